# Optimizing a Trainium2 kernel written in Bass

```python
import jax, jax.numpy as jnp
from jax import lax
import numpy as np

D_MODEL = 4096
BATCH = 4
SEQ = 2048
DEPTH = 1

GRID_W = 64
CTX_LEN = 256
N_MOD = 6
EPS = 1e-6
HEAD_DIM = 128
ATTN_HEADS = D_MODEL // (2 * HEAD_DIM)
ATTN_KV_HEADS = ATTN_HEADS // 4
GQA_GROUP = ATTN_HEADS // ATTN_KV_HEADS
ATTN_WIDTH = ATTN_HEADS * HEAD_DIM
KV_WIDTH = ATTN_KV_HEADS * HEAD_DIM
AXIS_DIM = HEAD_DIM // 2
ROPE_THETA = 10000.0
Q_BLOCK = 128
ATTN_SCALE = HEAD_DIM ** -0.5
HGRN_EXPAND = 128
HGRN_HEADS = D_MODEL // (2 * HGRN_EXPAND)
HGRN_WIDTH = HGRN_HEADS * HGRN_EXPAND
HGRN_HEAD_V = HGRN_WIDTH // HGRN_HEADS
CHUNK = 64
MIX_WIDTH = ATTN_WIDTH + HGRN_WIDTH
HEAD_COLS = ATTN_WIDTH + 2 * HGRN_WIDTH
TAIL_COLS = 2 * KV_WIDTH + 3 * HGRN_WIDTH
IN_WIDTH = HEAD_COLS + TAIL_COLS
HEAD_SPLITS = [ATTN_WIDTH, ATTN_WIDTH + HGRN_WIDTH]
TAIL_SPLITS = [KV_WIDTH, 2 * KV_WIDTH, 2 * KV_WIDTH + HGRN_WIDTH, 2 * KV_WIDTH + 2 * HGRN_WIDTH]
COL_SPLITS = HEAD_SPLITS + [HEAD_COLS + s for s in [0] + TAIL_SPLITS]
N_EXPERTS = 32
TOP_K = 4
D_EXPERT = 3 * D_MODEL // 8
SWIGLU_LIMIT = 7.0
SWIGLU_ALPHA = 1.702

kernel_name = "hybrid_attn_hgrn2_moe_dit_layer"


def rms_norm(x, gain):
    xf = x.astype(jnp.float32)
    y = xf * lax.rsqrt(jnp.mean(xf * xf, axis=-1, keepdims=True) + EPS)
    return (y * gain.astype(jnp.float32)).astype(x.dtype)


def modulate(x, shift, scale):
    return x * (1 + scale) + shift


def ada_modulation(cond, w_ada, b_ada):
    m = jax.nn.silu(cond) @ w_ada + b_ada
    return m.reshape(*cond.shape[:-1], N_MOD, D_MODEL)


def heads(a, n):
    return a.reshape(*a.shape[:-1], n, a.shape[-1] // n)


def flip(a):
    return jnp.flip(a, axis=1)


def axial_rope_tables(rows):
    row_ids = jnp.repeat(jnp.arange(rows), GRID_W).astype(jnp.float32)
    col_ids = jnp.tile(jnp.arange(GRID_W), rows).astype(jnp.float32)
    inv_freq = ROPE_THETA ** (-jnp.arange(0, AXIS_DIM, 2, dtype=jnp.float32) / AXIS_DIM)
    ang_r = row_ids[:, None] * inv_freq[None, :]
    ang_c = col_ids[:, None] * inv_freq[None, :]
    return (jnp.cos(ang_r), jnp.sin(ang_r), jnp.cos(ang_c), jnp.sin(ang_c))


def rotate_half(x, cos, sin):
    x1, x2 = jnp.split(x, 2, axis=-1)
    cos = cos[None, :, None, :]
    sin = sin[None, :, None, :]
    return jnp.concatenate([x1 * cos - x2 * sin, x1 * sin + x2 * cos], axis=-1)


def apply_axial_rope(x, rope):
    cos_r, sin_r, cos_c, sin_c = rope
    x_row, x_col = jnp.split(x.astype(jnp.float32), 2, axis=-1)
    out = jnp.concatenate([rotate_half(x_row, cos_r, sin_r), rotate_half(x_col, cos_c, sin_c)], axis=-1)
    return out.astype(x.dtype)


def attend(q, keys, vals):
    s = jnp.einsum('bqhgd,bkhd->bhgqk', q, keys, preferred_element_type=jnp.float32) * ATTN_SCALE
    p = jax.nn.softmax(s, axis=-1).astype(vals.dtype)
    return jnp.einsum('bhgqk,bkhd->bqhgd', p, vals)


def hgrn2_forget(f_raw, lower_bound):
    f = lower_bound + (1 - lower_bound) * jax.nn.sigmoid(f_raw.astype(jnp.float32))
    return heads(1 - f, HGRN_HEADS), heads(jnp.log(f), HGRN_HEADS)


def hgrn2_chunk_scan(k, v, log_f, s0, q=None):
    B, L, H, _ = k.shape
    nc = L // CHUNK
    blk = lambda a: a.astype(jnp.float32).reshape(B, nc, CHUNK, H, a.shape[-1])
    k, v, log_f = blk(k), blk(v), blk(log_f)
    b = jnp.cumsum(log_f, axis=2)
    b_last = b[:, :, -1]
    u = jnp.einsum('bnshk,bnshv->bnhkv', k * jnp.exp(b_last[:, :, None] - b), v)
    decay = jnp.exp(b_last)
    with_out = q is not None

    def step(state, inp):
        a_c, u_c = inp
        return a_c[..., None] * state + u_c, (state if with_out else None)

    s_final, s_start = lax.scan(step, s0, (jnp.moveaxis(decay, 1, 0), jnp.moveaxis(u, 1, 0)))
    if not with_out:
        return None, s_final
    q = blk(q)
    b_mid = b[:, :, CHUNK // 2 - 1][:, :, None]
    a = jnp.einsum('bnthk,bnshk->bnhts', q * jnp.exp(b - b_mid), k * jnp.exp(b_mid - b))
    within_chunk = jnp.tril(jnp.ones((CHUNK, CHUNK), dtype=bool))
    a = jnp.where(within_chunk, a, 0.0)
    o = (jnp.einsum('bnhts,bnshv->bnthv', a, v)
         + jnp.einsum('bnthk,bnhkv->bnthv', q * jnp.exp(b), jnp.moveaxis(s_start, 0, 1)))
    return o.reshape(B, L, H, v.shape[-1]), s_final


def hgrn2_readout(o_sum, gate_raw, hg_gain, dtype):
    o = rms_norm(o_sum, hg_gain).astype(dtype) * jax.nn.silu(heads(gate_raw, HGRN_HEADS))
    return o.reshape(*o.shape[:-2], HGRN_WIDTH)


def token_mixers(h, hc, rope, lb, need_ctx_out, w_in, q_gain, k_gain, hg_gain, w_out):
    B, L, _ = h.shape
    C = hc.shape[1]
    proj = h @ w_in
    q_a, hq, hg, k_a, v_a, hf_fw, hf_bw, hi = jnp.split(proj, COL_SPLITS, axis=-1)
    proj_c = hc @ w_in[:, HEAD_COLS:]
    kc_a, vc_a, hcf_fw, hcf_bw, hci = jnp.split(proj_c, TAIL_SPLITS, axis=-1)
    if need_ctx_out:
        qc_a, hcq, hcg = jnp.split(hc @ w_in[:, :HEAD_COLS], HEAD_SPLITS, axis=-1)

    q = apply_axial_rope(rms_norm(heads(q_a, ATTN_HEADS), q_gain), rope)
    k = apply_axial_rope(rms_norm(heads(k_a, ATTN_KV_HEADS), k_gain), rope)
    v = heads(v_a, ATTN_KV_HEADS)
    kc = rms_norm(heads(kc_a, ATTN_KV_HEADS), k_gain)
    vc = heads(vc_a, ATTN_KV_HEADS)
    keys = jnp.concatenate([kc, k], axis=1)
    vals = jnp.concatenate([vc, v], axis=1)
    qb = jnp.moveaxis(q.reshape(B, L // Q_BLOCK, Q_BLOCK, ATTN_KV_HEADS, GQA_GROUP, HEAD_DIM), 1, 0)
    attn = lax.map(lambda q_blk: attend(q_blk, keys, vals), qb)
    attn = jnp.moveaxis(attn, 0, 1).reshape(B, L, ATTN_WIDTH)

    s0 = jnp.zeros((B, HGRN_HEADS, HGRN_EXPAND, HGRN_HEAD_V), jnp.float32)
    kcf, lcf = hgrn2_forget(hcf_fw, lb[0])
    kcb, lcb = hgrn2_forget(hcf_bw, lb[1])
    vch = heads(hci, HGRN_HEADS)
    qch = jax.nn.silu(heads(hcq, HGRN_HEADS)) if need_ctx_out else None
    oc_f, sc_f = hgrn2_chunk_scan(kcf, vch, lcf, s0, qch)
    oc_b, sc_b = hgrn2_chunk_scan(flip(kcb), flip(vch), flip(lcb), s0,
                                  flip(qch) if need_ctx_out else None)
    kf, lf = hgrn2_forget(hf_fw, lb[0])
    kb, lbk = hgrn2_forget(hf_bw, lb[1])
    qh = jax.nn.silu(heads(hq, HGRN_HEADS))
    vh = heads(hi, HGRN_HEADS)
    o_f, _ = hgrn2_chunk_scan(kf, vh, lf, sc_f, qh)
    o_b, _ = hgrn2_chunk_scan(flip(kb), flip(vh), flip(lbk), sc_b, flip(qh))
    hgrn = hgrn2_readout(o_f + flip(o_b), hg, hg_gain, h.dtype)

    y = jnp.concatenate([attn, hgrn], axis=-1) @ w_out
    y_c = None
    if need_ctx_out:
        qc = rms_norm(heads(qc_a, ATTN_HEADS), q_gain).reshape(B, C, ATTN_KV_HEADS, GQA_GROUP, HEAD_DIM)
        attn_c = attend(qc, kc, vc).reshape(B, C, ATTN_WIDTH)
        hgrn_c = hgrn2_readout(oc_f + flip(oc_b), hcg, hg_gain, hc.dtype)
        y_c = jnp.concatenate([attn_c, hgrn_c], axis=-1) @ w_out
    return y, y_c


def moe_ffn(x, w_router, b_router, w_gate, b_gate, w_up, b_up, w_down, b_down):
    shape = x.shape
    xt = x.reshape(-1, shape[-1])
    logits = (xt @ w_router).astype(jnp.float32) + b_router.astype(jnp.float32)
    top_vals, top_idx = lax.top_k(logits, TOP_K)
    top_w = jax.nn.softmax(top_vals, axis=-1)
    combine = jnp.sum(jax.nn.one_hot(top_idx, N_EXPERTS, dtype=jnp.float32) * top_w[..., None], axis=1)
    out = jnp.zeros(xt.shape, jnp.float32)
    for e in range(N_EXPERTS):
        g = jnp.minimum(xt @ w_gate[e] + b_gate[e], SWIGLU_LIMIT)
        u = jnp.clip(xt @ w_up[e] + b_up[e], -SWIGLU_LIMIT, SWIGLU_LIMIT)
        act = g * jax.nn.sigmoid(SWIGLU_ALPHA * g) * (u + 1)
        out = out + combine[:, e:e + 1] * (act @ w_down[e] + b_down[e])
    return out.astype(x.dtype).reshape(shape)


def hybrid_layer(x, xc, c, c_ctx, rope, lb, need_ctx_out, w_ada, b_ada, gains, w_in, q_gain, k_gain,
                 hg_gain, w_out, w_router, b_router, w_gate, b_gate, w_up, b_up, w_down, b_down):
    mod = ada_modulation(c, w_ada, b_ada)
    mod_c = ada_modulation(c_ctx, w_ada, b_ada)
    sh1, sc1, g1, sh2, sc2, g2 = [mod[:, i, None, :] for i in range(N_MOD)]
    h = modulate(rms_norm(x, gains[0]), sh1, sc1)
    hc = modulate(rms_norm(xc, gains[0]), mod_c[0], mod_c[1])
    y, y_c = token_mixers(h, hc, rope, lb, need_ctx_out, w_in, q_gain, k_gain, hg_gain, w_out)
    x = x + g1 * rms_norm(y, gains[1])
    h2 = modulate(rms_norm(x, gains[2]), sh2, sc2)
    x = x + g2 * rms_norm(moe_ffn(h2, w_router, b_router, w_gate, b_gate, w_up, b_up, w_down, b_down), gains[3])
    if need_ctx_out:
        xc = xc + mod_c[2] * rms_norm(y_c, gains[1])
        hc2 = modulate(rms_norm(xc, gains[2]), mod_c[3], mod_c[4])
        xc = xc + mod_c[5] * rms_norm(moe_ffn(hc2, w_router, b_router, w_gate, b_gate, w_up, b_up, w_down, b_down), gains[3])
    return x, xc


def setup_inputs(seed: int = 0) -> dict:
    key = jax.random.key(seed)
    ks = jax.random.split(key, 24)
    f32 = jnp.float32
    nrm = lambda k, shape, s: jax.random.normal(k, shape, f32) * s
    D, L = D_MODEL, DEPTH
    return {
        "x": nrm(ks[0], (BATCH, SEQ, D), 1.0),
        "c": nrm(ks[1], (BATCH, D), 1.0),
        "ctx": nrm(ks[2], (BATCH, CTX_LEN, D), 1.0),
        "c_ctx": nrm(ks[3], (D,), 1.0),
        "w_ada": nrm(ks[4], (L, D, N_MOD * D), 0.5 * D ** -0.5),
        "b_ada": nrm(ks[5], (L, N_MOD * D), 0.02),
        "norm_gains": 1.0 + nrm(ks[6], (L, 4, D), 0.05),
        "w_in": nrm(ks[7], (L, D, IN_WIDTH), D ** -0.5),
        "q_norm_gain": 1.0 + nrm(ks[8], (L, HEAD_DIM), 0.05),
        "k_norm_gain": 1.0 + nrm(ks[9], (L, HEAD_DIM), 0.05),
        "hgrn_lb_logits": nrm(ks[10], (DEPTH + 1, 2, HGRN_WIDTH), 0.5),
        "hgrn_norm_gain": 1.0 + nrm(ks[11], (L, HGRN_HEAD_V), 0.05),
        "w_out": nrm(ks[12], (L, MIX_WIDTH, D), MIX_WIDTH ** -0.5),
        "w_router": nrm(ks[13], (L, D, N_EXPERTS), D ** -0.5),
        "b_router": nrm(ks[14], (L, N_EXPERTS), 0.01),
        "w_gate": nrm(ks[15], (L, N_EXPERTS, D, D_EXPERT), D ** -0.5),
        "b_gate": nrm(ks[16], (L, N_EXPERTS, D_EXPERT), 0.02),
        "w_up": nrm(ks[17], (L, N_EXPERTS, D, D_EXPERT), D ** -0.5),
        "b_up": nrm(ks[18], (L, N_EXPERTS, D_EXPERT), 0.02),
        "w_down": nrm(ks[19], (L, N_EXPERTS, D_EXPERT, D), D_EXPERT ** -0.5),
        "b_down": nrm(ks[20], (L, N_EXPERTS, D), 0.02),
    }


def reference(x, c, ctx, c_ctx, w_ada, b_ada, norm_gains, w_in, q_norm_gain, k_norm_gain,
              hgrn_lb_logits, hgrn_norm_gain, w_out, w_router, b_router,
              w_gate, b_gate, w_up, b_up, w_down, b_down):
    rows = x.shape[1] // GRID_W
    rope = axial_rope_tables(rows)
    lower_bounds = jnp.cumsum(jax.nn.softmax(hgrn_lb_logits.astype(jnp.float32), axis=0), axis=0)
    xc = ctx
    for layer in range(DEPTH):
        need_ctx_out = layer + 1 < DEPTH
        x, xc = hybrid_layer(x, xc, c, c_ctx, rope, lower_bounds[layer], need_ctx_out,
                             w_ada[layer], b_ada[layer], norm_gains[layer], w_in[layer],
                             q_norm_gain[layer], k_norm_gain[layer], hgrn_norm_gain[layer], w_out[layer],
                             w_router[layer], b_router[layer], w_gate[layer], b_gate[layer],
                             w_up[layer], b_up[layer], w_down[layer], b_down[layer])
    return x
```

```python
import numpy as np
import concourse.bass as bass
import concourse.mybir as mybir
from concourse.bass_utils import run_bass_kernel_spmd

F32 = mybir.dt.float32
BF16 = mybir.dt.bfloat16
ALU = mybir.AluOpType
ACT = mybir.ActivationFunctionType
AX = mybir.AxisListType
NCORES = 8


class Cfg:
    def __init__(self, D=4096, B=4, SEQ=2048, CTX=256, NE=32, GRID_W=64):
        self.D, self.B, self.SEQ, self.CTX, self.NE, self.GRID_W = D, B, SEQ, CTX, NE, GRID_W
        self.KC = D // 128
        self.HALF = SEQ // 2
        self.AH = D // 256
        self.KVH = self.AH // 4
        self.HH = D // 256
        self.AW = self.AH * 128
        self.KVW = self.KVH * 128
        self.HW = self.HH * 128
        self.MIXW = self.AW + self.HW
        self.DE = 3 * D // 8
        self.JE = self.DE // 128
        self.NTOK = 2 * self.HALF + CTX
        self.NT = self.NTOK // 128
        self.TO = self.HALF // 128
        self.TC = CTX // 128
        self.NMOD = 6 * D
        self.NMODC = self.NMOD // NCORES
        self.EL = NE // NCORES
        self.EPS = 1e-6
        self.TOPK = 4


class TT:
    def __init__(self, h, name):
        self.h = h
        self.name = name
        self.w = None
        self.r = []

    def __getitem__(self, k):
        return self.h[k]


class Prog:
    COMPUTE = ("pe", "dve", "act", "pool")

    def __init__(self, nc, ndma=10):
        self.nc = nc
        self.q = {e: [] for e in ("pe", "dve", "act", "pool", "sp")}
        self.sems = {}
        self.cnt = {}
        self.seen = {e: {} for e in self.q}
        self.ndma = ndma
        self.dma_rr = 0
        self.cc_n = 0
        self._stack = []
        self._semstack = []
        for i in range(15):
            self.sem(("cc", i))
        self.bg_rr = 0
        for i in range(3):
            self.sem(("b", i))
        for k in ("pe", "dve", "act", "pool"):
            self.sem(k)
        for i in range(ndma):
            self.sem(("d", i))

    def sem(self, key):
        if key not in self.sems:
            cm = self.nc.semaphore("s_" + "_".join(str(k) for k in (key if isinstance(key, tuple) else (key,))))
            self.sems[key] = cm.__enter__()
            self._semstack.append(cm)
            self.cnt[key] = 0
        return self.sems[key]

    def sb(self, name, shape, dt):
        cm = self.nc.sbuf_tensor("sb_" + name, list(shape), dt)
        h = cm.__enter__()
        self._stack.append(cm)
        return TT(h, name)

    def ps(self, name, shape, dt=F32):
        cm = self.nc.psum_tensor("ps_" + name, list(shape), dt)
        h = cm.__enter__()
        self._stack.append(cm)
        return TT(h, name)

    def dram(self, name, shape, dt, kind=None):
        if kind is None:
            h = self.nc.dram_tensor(name, list(shape), dt)
        else:
            h = self.nc.dram_tensor(name, list(shape), dt, kind=kind)
        return TT(h, name)

    def mark(self):
        return len(self._stack)

    def release(self, mark):
        while len(self._stack) > mark:
            self._stack.pop().__exit__(None, None, None)

    def mm(self, out_t, items, reads):
        waits = self._deps("pe", reads, [out_t])
        s = self.sem("pe")
        self.cnt["pe"] += 1
        self._commit(("pe", self.cnt["pe"]), reads, [out_t])
        wl = [(self.sem(k), v) for k, v in waits]

        def run(e):
            for sm, v in wl:
                e.wait_ge(sm, v)
            for i, it in enumerate(items):
                ins = e.matmul(**it)
                if i == len(items) - 1:
                    ins.then_inc(s, 1)
        self.q["pe"].append(run)

    def _deps(self, eng, reads, writes):
        need = {}

        def add(kv):
            if kv is None:
                return
            k, v = kv
            if k == eng and eng == "pe":
                return
            if need.get(k, 0) < v:
                need[k] = v
        for t in reads:
            add(t.w)
        for t in writes:
            add(t.w)
            for kv in t.r:
                add(kv)
        out = []
        for k, v in need.items():
            if self.seen[eng].get(k, 0) >= v:
                continue
            self.seen[eng][k] = v
            out.append((k, v))
        return out

    def _commit(self, token, reads, writes):
        for t in reads:
            t.r.append(token)
            if len(t.r) > 12:
                m = {}
                for k, v in t.r:
                    m[k] = max(m.get(k, 0), v)
                t.r = list(m.items())
        for t in writes:
            t.w = token
            t.r = []

    def op(self, eng, fn, reads=(), writes=(), inc=True):
        waits = self._deps(eng, reads, writes)
        s = self.sem(eng)
        if inc:
            self.cnt[eng] += 1
            token = (eng, self.cnt[eng])
        else:
            token = (eng, self.cnt[eng] + 1)
        self._commit(token, reads, writes)
        wl = [(self.sem(k), v) for k, v in waits]

        def run(e):
            for sm, v in wl:
                e.wait_ge(sm, v)
            ins = fn(e)
            if inc:
                ins.then_inc(s, 1)
        self.q[eng].append(run)

    def dma(self, eng, out_t, out_ap, in_t, in_ap, bg=False, **kw):
        if bg:
            slot = ("b", self.bg_rr % 3)
            self.bg_rr += 1
        else:
            slot = ("d", self.dma_rr % self.ndma)
            self.dma_rr += 1
        s = self.sem(slot)
        waits = self._deps(eng, [in_t], [out_t])
        prev = self.cnt[slot]
        if prev > 0 and self.seen[eng].get(slot, 0) < prev:
            waits.append((slot, prev))
            self.seen[eng][slot] = prev
        self.cnt[slot] += 16
        token = (slot, self.cnt[slot])
        self._commit(token, [in_t], [out_t])
        wl = [(self.sem(k), v) for k, v in waits]

        def run(e):
            for sm, v in wl:
                e.wait_ge(sm, v)
            e.dma_start(out=out_ap, in_=in_ap, **kw).then_inc(s, 16)
        self.q[eng].append(run)

    def allgather(self, in_t, out_t):
        key = ("cc", self.cc_n)
        self.cc_n += 1
        s = self.sem(key)
        waits = self._deps("pool", [in_t], [out_t])
        CCV = 1
        if self.cc_n > 1:
            pk = ("cc", self.cc_n - 2)
            if self.seen["pool"].get(pk, 0) < CCV:
                self.seen["pool"][pk] = CCV
                waits.append((pk, CCV))
        self.cnt[key] = CCV
        self._commit((key, CCV), [in_t], [out_t])
        wl = [(self.sem(k), v) for k, v in waits]
        i_ap, o_ap = in_t.h.ap().opt(), out_t.h.ap().opt()

        def run(e):
            for sm, v in wl:
                e.wait_ge(sm, v)
            e.collective_compute("AllGather", ALU.bypass, replica_groups=[list(range(NCORES))],
                                 ins=[i_ap], outs=[o_ap]).then_inc(s)
        self.q["pool"].append(run)

    def wait_all(self, eng, tts):
        waits = self._deps(eng, tts, [])
        wl = [(self.sem(k), v) for k, v in waits]

        def run(e):
            for sm, v in wl:
                e.wait_ge(sm, v)
        self.q[eng].append(run)

    def final_wait(self, eng):
        wl = []
        for k, v in self.cnt.items():
            if v > 0 and self.seen[eng].get(k, 0) < v:
                self.seen[eng][k] = v
                wl.append((self.sem(k), v))

        def run(e):
            for sm, v in wl:
                e.wait_ge(sm, v)
        self.q[eng].append(run)

    def barrier(self):
        snap = {k: v for k, v in self.cnt.items() if v > 0 and not (isinstance(k, tuple) and k[0] in ("cc", "b"))}
        for eng in self.q:
            wl = []
            for k, v in snap.items():
                if k == eng and eng == "pe":
                    continue
                if self.seen[eng].get(k, 0) < v:
                    self.seen[eng][k] = v
                    wl.append((self.sem(k), v))

            def run(e, wl=wl):
                for sm, v in wl:
                    e.wait_ge(sm, v)
            self.q[eng].append(run)

    def emit(self):
        nc = self.nc
        with nc.Block() as block:
            @block.tensor
            def _(e):
                for f in self.q["pe"]:
                    f(e)

            @block.vector
            def _(e):
                for f in self.q["dve"]:
                    f(e)

            @block.scalar
            def _(e):
                for f in self.q["act"]:
                    f(e)

            @block.gpsimd
            def _(e):
                for f in self.q["pool"]:
                    f(e)

            @block.sync
            def _(e):
                for f in self.q["sp"]:
                    f(e)

    def close(self):
        while self._stack:
            self._stack.pop().__exit__(None, None, None)
        while self._semstack:
            self._semstack.pop().__exit__(None, None, None)


class IO:
    pass


def col_map(cfg):
    AW, HW, KVW = cfg.AW, cfg.HW, cfg.KVW
    HC = AW + 2 * HW
    return dict(q=0, hq=AW, hg=AW + HW, k=HC, v=HC + KVW, ffw=HC + 2 * KVW,
                fbw=HC + 2 * KVW + HW, hi=HC + 2 * KVW + 2 * HW, INW=HC + 2 * KVW + 3 * HW)


def build(cfg, stop_after=99, debug=()):
    nc = bass.Bass("TRN2", target_bir_lowering=False)
    P = Prog(nc)
    D, KC, HALF, CTX, NT, TO, TC = cfg.D, cfg.KC, cfg.HALF, cfg.CTX, cfg.NT, cfg.TO, cfg.TC
    AH, KVH, HH, AW, KVW, HW, MIXW = cfg.AH, cfg.KVH, cfg.HH, cfg.AW, cfg.KVW, cfg.HW, cfg.MIXW
    NE, EL, DE, JE, NMODC = cfg.NE, cfg.EL, cfg.DE, cfg.JE, cfg.NMODC
    cm = col_map(cfg)
    INW = cm["INW"]
    NB = D // 512 if D >= 512 else 1
    io = IO()

    def ext(name, shape, dt=F32):
        t = P.dram(name, shape, dt, kind="ExternalInput")
        setattr(io, name, t)
        return t
    ext("x_oth", [HALF, D]); ext("x_own", [HALF, D]); ext("ctx_b", [CTX, D])
    ext("c5T", [128, KC * 5]); ext("selm", [40, 16])
    ext("w_ada_s", [D, NMODC]); ext("b_ada_s", [1, NMODC]); ext("gainsT", [128, 4 * KC])
    ext("w_in_s", [D // 8, INW]); ext("w_out_s", [MIXW // 8, D])
    ext("ropeC", [2 * HALF, 128]); ext("ropeS", [2 * HALF, 128])
    ext("qk_gain", [2, 128]); ext("hg_gain", [1, 128])
    ext("lbT", [1, 4 * HW])
    ext("flags", [1, 2])
    ext("ident", [128, 128]); ext("hmats", [64, 6 * 64]); ext("amask", [64, 64])
    ext("w_rt", [128, KC * NE]); ext("b_rt", [1, NE])
    ext("wg_s", [EL * JE * 128, D]); ext("wu_s", [EL * JE * 128, D]); ext("wd_s", [EL * NB * 128, JE * 512])
    ext("bgT", [128, NE * JE]); ext("buT", [128, NE * JE]); ext("b_dn", [NE, D])
    out = P.dram("out", [HALF, D], F32, kind="ExternalOutput")
    dbg = {}

    def dbg_out(name, shape, dt=F32):
        if name in debug:
            dbg[name] = P.dram("dbg_" + name, shape, dt, kind="ExternalOutput")
            return dbg[name]
        return None

    w_in_b = P.dram("w_in_b", [D // 8, INW], BF16); w_in_f = P.dram("w_in_f", [D, INW], BF16)
    w_out_b = P.dram("w_out_b", [MIXW // 8, D], BF16); w_out_f = P.dram("w_out_f", [MIXW, D], BF16)
    RJ = JE * 128
    eg_b = [P.dram(f"eg_b{i}", [RJ, D], BF16) for i in range(EL)]
    eg_f = [P.dram(f"eg_f{i}", [8 * RJ, D], BF16) for i in range(EL)]
    eu_b = [P.dram(f"eu_b{i}", [RJ, D], BF16) for i in range(EL)]
    eu_f = [P.dram(f"eu_f{i}", [8 * RJ, D], BF16) for i in range(EL)]
    ed_b = [P.dram(f"ed_b{i}", [RJ, D], BF16) for i in range(EL)]
    ed_f = [P.dram(f"ed_f{i}", [8 * RJ, D], BF16) for i in range(EL)]

    def wd_view(t, row0):
        return t[row0:row0 + RJ, :].rearrange("r c -> (r c)").rearrange("(n m) -> n m", m=JE * 512)
    modp_d = P.dram("modp_d", [5, NMODC], F32); mod_all = P.dram("mod_all", [40, NMODC], F32)
    g_d = P.dram("g_d", [1, 2 * D], F32)
    lb_d = P.dram("lb_d", [1, 4 * HW], F32)
    hT_d = P.dram("hT_d", [NT, 128, KC * 128], BF16)
    qT_d = P.dram("qT_d", [AH, 128, HALF], BF16)
    kT_d = P.dram("kT_d", [KVH, 128, cfg.NTOK], BF16)
    v_d = P.dram("v_d", [NT, 128, KVH * 129], BF16)
    fr_d = P.dram("fr_d", [NT, 128, 2 * HW], F32)
    vi_d = P.dram("vi_d", [NT, 128, HW], BF16)
    hg_d = P.dram("hg_d", [TO, 128, HW], F32)
    qh_d = P.dram("qh_d", [HH, 128, HALF], F32)
    of_d = P.dram("of_d", [HALF, HW], F32)
    mix_d = P.dram("mix_d", [TO, 128, MIXW], F32)
    y_d = P.dram("y_d", [TO, 128, D], F32)
    x1_d = P.dram("x1_d", [TO, 128, D], F32)
    h2T_d = P.dram("h2T_d", [TO, 128, KC * 128], BF16)

    ident = P.sb("ident", [128, 128], F32)
    P.dma("sp", ident, ident[:], io.ident, io.ident[:])
    epsc = P.sb("epsc", [128, 1], F32)
    P.op("dve", lambda e: e.memset(epsc[:], cfg.EPS), [], [epsc])
    coef = P.sb("coef", [128, 8, KC], F32)
    cmb = P.sb("cmb", [128, TO, NE], F32)
    persist_mark = P.mark()

    def cast_rows(src, dst, rows, step=512):
        for r0 in range(0, rows, step):
            r1 = min(rows, r0 + step)
            P.dma("pool", dst, dst[r0:r1, :], src, src[r0:r1, :], bg=True)

    cast_rows(io.w_in_s, w_in_b, D // 8)
    P.allgather(w_in_b, w_in_f)

    def stage_adaln():
        c5 = P.sb("c5", [128, KC, 5], F32)
        P.dma("sp", c5, c5[:], io.c5T, io.c5T[:].rearrange("p (k r) -> p k r", r=5))
        c5s = P.sb("c5s", [128, KC, 5], BF16)
        P.op("act", lambda e: e.activation(out=c5s[:], in_=c5[:], func=ACT.Silu), [c5], [c5s])
        bada = P.sb("bada", [5, NMODC], F32)
        P.dma("sp", bada, bada[:], io.b_ada_s, io.b_ada_s[0:1, :].partition_broadcast(5))
        modp = P.sb("modp", [5, NMODC], F32)
        wb = [P.sb(f"wada{i}", [128, KC, 512], BF16) for i in range(2)]
        pa = [P.ps(f"pada{i}", [128, 512]) for i in range(2)]
        wsrc = io.w_ada_s[:].rearrange("(k p) n -> p k n", p=128)
        bw = 512 if NMODC % 512 == 0 else 256
        nblk = NMODC // bw
        for nb in range(nblk):
            w = wb[nb % 2]; ps = pa[nb % 2]
            for k0 in range(0, KC, 8):
                P.dma("pool", w, w[:, k0:k0 + 8, 0:bw], io.w_ada_s, wsrc[:, k0:k0 + 8, nb * bw:(nb + 1) * bw])
            P.mm(ps, [dict(out=ps[0:5, 0:bw], lhsT=c5s[:, k, :], rhs=w[:, k, 0:bw], start=(k == 0), stop=(k == KC - 1))
                      for k in range(KC)], [c5s, w])
            P.op("dve", lambda e, ps=ps, nb=nb: e.tensor_tensor(out=modp[:, nb * bw:(nb + 1) * bw], in0=ps[0:5, 0:bw],
                                                                 in1=bada[:, nb * bw:(nb + 1) * bw], op=ALU.add),
                 [ps, bada], [modp])
        P.dma("sp", modp_d, modp_d[:, :], modp, modp[:])
        P.allgather(modp_d, mod_all)
        rows = P.sb("modrows", [40, NMODC], F32)
        P.dma("sp", rows, rows[:], mod_all, mod_all[:, :])
        selm = P.sb("selm", [40, 16], F32)
        P.dma("sp", selm, selm[:], io.selm, io.selm[:])
        J = NMODC // 128
        psm = P.ps("psm", [128, J, 16])
        P.mm(psm, [dict(out=psm[:, j, :], lhsT=rows[:, j * 128:(j + 1) * 128], rhs=selm[:, :], start=True, stop=True)
                   for j in range(J)], [rows, selm])
        modT = P.sb("modT", [128, 8 * J], F32)
        modcT = P.sb("modcT", [128, 8 * J], F32)
        P.op("dve", lambda e: e.tensor_copy(out=modT[:].rearrange("p (r j) -> p j r", j=J), in_=psm[:, :, 0:8]), [psm], [modT])
        P.op("dve", lambda e: e.tensor_copy(out=modcT[:].rearrange("p (r j) -> p j r", j=J), in_=psm[:, :, 8:16]), [psm], [modcT])
        gn = P.sb("gainsT", [128, 4, KC], F32)
        P.dma("sp", gn, gn[:], io.gainsT, io.gainsT[:].rearrange("p (g k) -> p g k", k=KC))

        def m(t, i):
            return t[:, i * KC:(i + 1) * KC]
        def stt(o, a, b):
            P.op("dve", lambda e: e.scalar_tensor_tensor(out=o, in0=a, scalar=1.0, in1=b, op0=ALU.add, op1=ALU.mult),
                 [modT, modcT, gn], [coef])
        stt(coef[:, 0, :], m(modT, 1), gn[:, 0, :])
        P.op("dve", lambda e: e.tensor_copy(out=coef[:, 1, :], in_=m(modT, 0)), [modT], [coef])
        stt(coef[:, 2, :], m(modcT, 1), gn[:, 0, :])
        P.op("dve", lambda e: e.tensor_copy(out=coef[:, 3, :], in_=m(modcT, 0)), [modcT], [coef])
        stt(coef[:, 4, :], m(modT, 4), gn[:, 2, :])
        P.op("dve", lambda e: e.tensor_copy(out=coef[:, 5, :], in_=m(modT, 3)), [modT], [coef])
        P.op("dve", lambda e: e.tensor_tensor(out=coef[:, 6, :], in0=m(modT, 2), in1=gn[:, 1, :], op=ALU.mult), [modT, gn], [coef])
        P.op("dve", lambda e: e.tensor_tensor(out=coef[:, 7, :], in0=m(modT, 5), in1=gn[:, 3, :], op=ALU.mult), [modT, gn], [coef])
        pg = P.ps("pg", [KC, 2, 128])
        P.mm(pg, [dict(out=pg[:, i, :], lhsT=coef[:, 6 + i, :], rhs=ident[:, :], start=True, stop=True) for i in range(2)],
             [coef, ident])
        gs = P.sb("gs", [KC, 2, 128], F32)
        P.op("dve", lambda e: e.tensor_copy(out=gs[:], in_=pg[:]), [pg], [gs])
        for i in range(2):
            P.dma("sp", g_d, g_d[0:1, i * D:(i + 1) * D].rearrange("o (k c) -> (o k) c", c=128), gs, gs[:, i, :])
        d = dbg_out("coef", [128, 8 * KC])
        if d is not None:
            P.dma("sp", d, d[:, :], coef, coef[:].rearrange("p a k -> p (a k)"))

    mk = P.mark()
    stage_adaln()
    P.barrier()
    P.release(mk)

    def norm_transpose(tag, src_t, src_ap, ntiles, ai, bi, dst_t, dst0, do_norm=True, W=D, hook=None):
        WC = W // 128
        xt = [P.sb(f"{tag}_x{i}", [128, W], F32) for i in range(2)]
        junk = P.sb(f"{tag}_junk", [128, W], BF16)
        ss = [P.sb(f"{tag}_ss{i}", [128, 2], F32) for i in range(2)]
        dg = [P.sb(f"{tag}_dg{i}", [128, 128], F32) for i in range(2)]
        hs = [P.sb(f"{tag}_h{i}", [128, WC, 128], BF16) for i in range(2)]
        pss = [P.ps(f"{tag}_ps{i}", [128, 512]) for i in range(3)]
        pi = 0
        for t in range(ntiles):
            x = xt[t % 2]; s = ss[t % 2]; g = dg[t % 2]; h = hs[t % 2]
            P.dma("sp", x, x[:], src_t, src_ap(t))
            if do_norm:
                P.op("dve", lambda e, s=s: e.memset(s[:], 0.0), [], [s])
                P.op("act", lambda e, x=x, s=s: e.activation(out=junk[:], in_=x[:], func=ACT.Square, accum_out=s[:, 0:1]),
                     [x, s], [junk, s])
                P.op("act", lambda e, s=s: e.activation(out=s[:, 1:2], in_=s[:, 0:1], func=ACT.Sqrt, scale=1.0 / W, bias=epsc[:, 0:1]), [s, epsc], [s])
                P.op("dve", lambda e, s=s: e.reciprocal(out=s[:, 1:2], in_=s[:, 1:2]), [s], [s])
                P.op("dve", lambda e, s=s, g=g: e.tensor_scalar(out=g[:], in0=ident[:], scalar1=s[:, 1:2], scalar2=None,
                                                                op0=ALU.mult), [s, ident], [g])
                rhs_t = g
            else:
                rhs_t = ident
            for c0 in range(0, WC, 4):
                ps = pss[pi % 3]; pi += 1
                n = min(4, WC - c0)
                P.mm(ps, [dict(out=ps[:, i * 128:(i + 1) * 128], lhsT=x[:, (c0 + i) * 128:(c0 + i + 1) * 128],
                               rhs=rhs_t[:, :], start=True, stop=True) for i in range(n)], [x, rhs_t])
                for i in range(n):
                    c = c0 + i
                    if ai is None:
                        fn = lambda e, ps=ps, i=i, c=c, h=h: e.tensor_copy(out=h[:, c, :], in_=ps[:, i * 128:(i + 1) * 128])
                        P.op("dve" if i % 2 else "act", (lambda e, ps=ps, i=i, c=c, h=h: e.copy(out=h[:, c, :], in_=ps[:, i * 128:(i + 1) * 128])) if i % 2 == 0 else fn,
                             [ps], [h])
                    elif i % 2 == 0:
                        P.op("act", lambda e, ps=ps, i=i, c=c, h=h: e.activation(
                            out=h[:, c, :], in_=ps[:, i * 128:(i + 1) * 128], func=ACT.Identity,
                            scale=coef[:, ai, c:c + 1], bias=coef[:, bi, c:c + 1]), [ps, coef], [h])
                    else:
                        P.op("dve", lambda e, ps=ps, i=i, c=c, h=h: e.tensor_scalar(
                            out=h[:, c, :], in0=ps[:, i * 128:(i + 1) * 128], scalar1=coef[:, ai, c:c + 1],
                            scalar2=coef[:, bi, c:c + 1], op0=ALU.mult, op1=ALU.add), [ps, coef], [h])
                if hook is not None:
                    hook(t, c0, n, ps)
            P.dma("sp", dst_t, dst_t[dst0 + t, :, :], h, h[:].rearrange("p c t -> p (c t)"))

    mk = P.mark()
    norm_transpose("n1a", io.x_oth, lambda t: io.x_oth[t * 128:(t + 1) * 128, :], TO, 0, 1, hT_d, 0)
    P.barrier(); P.release(mk); mk = P.mark()
    norm_transpose("n1b", io.x_own, lambda t: io.x_own[t * 128:(t + 1) * 128, :], TO, 0, 1, hT_d, TO)
    P.barrier(); P.release(mk); mk = P.mark()
    norm_transpose("n1c", io.ctx_b, lambda t: io.ctx_b[t * 128:(t + 1) * 128, :], TC, 2, 3, hT_d, 2 * TO)
    P.barrier(); P.release(mk)
    if "no_wout" not in debug:
        cast_rows(io.w_out_s, w_out_b, MIXW // 8)
        P.allgather(w_out_b, w_out_f)
    for i in range(EL if "no_exp" not in debug else 0):
        for (b_, f_, s_) in ((eg_b[i], eg_f[i], io.wg_s), (eu_b[i], eu_f[i], io.wu_s)):
            for q0 in range(0, RJ, 512):
                q1 = min(RJ, q0 + 512)
                P.dma("pool", b_, b_[q0:q1, :], s_, s_[i * RJ + q0:i * RJ + q1, :], bg=True)
            P.allgather(b_, f_)
        d_ = ed_b[i]
        wv = wd_view(d_, 0)
        for q0 in range(0, NB * 128, 512):
            q1 = min(NB * 128, q0 + 512)
            P.dma("pool", d_, wv[q0:q1, :], io.wd_s, io.wd_s[i * NB * 128 + q0:i * NB * 128 + q1, :], bg=True)
        P.allgather(d_, ed_f[i])
    d = dbg_out("hT", [NT, 128, KC * 128], BF16)
    if d is not None:
        P.dma("sp", d, d[:, :, :], hT_d, hT_d[:, :, :])
        P.barrier()

    if stop_after <= 2:
        return finish(nc, P, io, out, dbg)

    def stage_inproj(tag, tiles, segs, rope_tile0):
        G = len(tiles)
        hts = []
        for i, tl in enumerate(tiles):
            h = P.sb(f"{tag}_hT{i}", [128, KC, 128], BF16)
            P.dma("sp", h, h[:], hT_d, hT_d[tl, :, :].rearrange("p (k t) -> p k t", t=128))
            hts.append(h)
        wb = [P.sb(f"{tag}_w{i}", [128, KC, 512], BF16) for i in range(2)]
        pg = [P.ps(f"{tag}_pg{i}", [128, 512]) for i in range(4)]
        pt = [P.ps(f"{tag}_pt{i}", [128, 512]) for i in range(2)]
        raw = [P.sb(f"{tag}_raw{i}", [128, 512], F32) for i in range(2)]
        sq = P.sb(f"{tag}_sq", [128, 512], F32)
        qn = [P.sb(f"{tag}_qn{i}", [128, 512], F32) for i in range(2)]
        t2 = P.sb(f"{tag}_t2", [128, 512], F32)
        st = [P.sb(f"{tag}_st{i}", [128, 4], F32) for i in range(2)]
        trs = [P.sb(f"{tag}_trs{i}", [128, 4, 128], BF16) for i in range(2)]
        trf = [P.sb(f"{tag}_trf{i}", [128, 4, 128], F32) for i in range(2)]
        obf = [P.sb(f"{tag}_obf{i}", [128, 512], BF16) for i in range(2)]
        vst = [P.sb(f"{tag}_vst{i}", [128, 4, 129], BF16) for i in range(2)]
        for v in vst:
            P.op("dve", lambda e, v=v: e.memset(v[:], 1.0), [], [v])
        gbc = P.sb(f"{tag}_gbc", [128, 2, 128], F32)
        for i in range(2):
            P.dma("sp", gbc, gbc[:, i, :], io.qk_gain, io.qk_gain[i:i + 1, :].partition_broadcast(128))
        if rope_tile0 is not None:
            rc = P.sb(f"{tag}_rc", [128, G, 128], F32)
            rs = P.sb(f"{tag}_rs", [128, G, 128], F32)
            for i in range(G):
                r0 = (rope_tile0 + i) * 128
                P.dma("sp", rc, rc[:, i, :], io.ropeC, io.ropeC[r0:r0 + 128, :])
                P.dma("sp", rs, rs[:, i, :], io.ropeS, io.ropeS[r0:r0 + 128, :])
        cnt = {"b": 0}

        def transpose_out(srcf, nh, dst_t, dst_ap, as_bf16):
            k = cnt["b"] % 2
            ps = pt[k]
            P.mm(ps, [dict(out=ps[:, h * 128:(h + 1) * 128], lhsT=srcf[:, h * 128:(h + 1) * 128], rhs=ident[:, :],
                           start=True, stop=True) for h in range(nh)], [srcf, ident])
            o = trs[k] if as_bf16 else trf[k]
            P.op("act", lambda e: e.copy(out=o[:, 0:nh, :], in_=ps[:, 0:nh * 128].rearrange("p (h t) -> p h t", t=128)), [ps], [o])
            P.dma("sp", dst_t, dst_ap, o, o[:, 0:nh, :])

        def epi_qk(which, dst_t, dst_ap_fn, use_rope):
            def epi(i, tl, c0, cw, ps):
                nh = cw // 128
                k = cnt["b"] % 2
                r = raw[k]; q = qn[k]; s = st[k]
                P.op("act", lambda e: e.copy(out=r[:, 0:cw], in_=ps[:, 0:cw]), [ps], [r])
                P.op("dve", lambda e: e.tensor_tensor(out=sq[:, 0:cw], in0=r[:, 0:cw], in1=r[:, 0:cw], op=ALU.mult), [r], [sq])
                P.op("dve", lambda e: e.tensor_reduce(out=s[:, 0:nh], in_=sq[:, 0:cw].rearrange("p (h d) -> p h d", d=128),
                                                      axis=AX.X, op=ALU.add), [sq], [s])
                P.op("act", lambda e: e.activation(out=s[:, 0:nh], in_=s[:, 0:nh], func=ACT.Sqrt, scale=1.0 / 128, bias=epsc[:, 0:1]), [s, epsc], [s])
                P.op("dve", lambda e: e.reciprocal(out=s[:, 0:nh], in_=s[:, 0:nh]), [s], [s])
                for h in range(nh):
                    hs = slice(h * 128, (h + 1) * 128)
                    P.op("dve", lambda e, h=h, hs=hs: e.scalar_tensor_tensor(out=q[:, hs], in0=r[:, hs], scalar=s[:, h:h + 1],
                                                                               in1=gbc[:, which, :], op0=ALU.mult, op1=ALU.mult),
                         [r, s, gbc], [q])
                if use_rope:
                    for h in range(nh):
                        hs = slice(h * 128, (h + 1) * 128)
                        qv = q[:, hs].rearrange("p (a b c) -> p a b c", a=2, b=2)
                        tv = t2[:, hs].rearrange("p (a b c) -> p a b c", a=2, b=2)
                        sv = rs[:, i, :].rearrange("p (a b c) -> p a b c", a=2, b=2)
                        P.op("dve", lambda e, qv=qv, tv=tv, sv=sv: e.tensor_tensor(out=tv[:, :, 0, :], in0=qv[:, :, 1, :], in1=sv[:, :, 0, :], op=ALU.mult), [q, rs], [t2])
                        P.op("dve", lambda e, qv=qv, tv=tv, sv=sv: e.tensor_tensor(out=tv[:, :, 1, :], in0=qv[:, :, 0, :], in1=sv[:, :, 1, :], op=ALU.mult), [q, rs], [t2])
                        P.op("dve", lambda e, hs=hs: e.tensor_tensor(out=q[:, hs], in0=q[:, hs], in1=rc[:, i, :], op=ALU.mult), [q, rc], [q])
                        P.op("dve", lambda e, hs=hs: e.tensor_tensor(out=q[:, hs], in0=q[:, hs], in1=t2[:, hs], op=ALU.add), [q, t2], [q])
                transpose_out(q, nh, dst_t, dst_ap_fn(i, tl, c0, nh), True)
                cnt["b"] += 1
            return epi

        def epi_v(i, tl, c0, cw, ps):
            nh = cw // 128
            k = cnt["b"] % 2
            v = vst[k]
            P.op("act", lambda e: e.copy(out=v[:, 0:nh, 0:128], in_=ps[:, 0:cw].rearrange("p (h d) -> p h d", d=128)), [ps], [v])
            h0 = c0 // 128
            P.dma("sp", v_d, v_d[tl, :, h0 * 129:(h0 + nh) * 129], v, v[:, 0:nh, :].rearrange("p h d -> p (h d)"))
            cnt["b"] += 1

        def epi_store(dst_t, dst_ap_fn, bf):
            def epi(i, tl, c0, cw, ps):
                k = cnt["b"] % 2
                o = obf[k] if bf else raw[k]
                P.op("act" if cnt["b"] % 4 < 2 else "dve",
                     (lambda e: e.copy(out=o[:, 0:cw], in_=ps[:, 0:cw])) if cnt["b"] % 4 < 2 else
                     (lambda e: e.tensor_copy(out=o[:, 0:cw], in_=ps[:, 0:cw])), [ps], [o])
                P.dma("sp", dst_t, dst_ap_fn(i, tl, c0, cw), o, o[:, 0:cw])
                cnt["b"] += 1
            return epi

        def epi_hq(i, tl, c0, cw, ps):
            nh = cw // 128
            k = cnt["b"] % 2
            r = raw[k]
            P.op("act", lambda e: e.activation(out=r[:, 0:cw], in_=ps[:, 0:cw], func=ACT.Silu), [ps], [r])
            h0 = c0 // 128
            transpose_out(r, nh, qh_d, qh_d[h0:h0 + nh, :, i * 128:(i + 1) * 128].rearrange("h p t -> p h t"), False)
            cnt["b"] += 1

        epis = dict(
            q=epi_qk(0, qT_d, lambda i, tl, c0, nh: qT_d[c0 // 128:c0 // 128 + nh, :, i * 128:(i + 1) * 128].rearrange("h p t -> p h t"), True),
            k=epi_qk(1, kT_d, lambda i, tl, c0, nh: kT_d[c0 // 128:c0 // 128 + nh, :, tl * 128:(tl + 1) * 128].rearrange("h p t -> p h t"), True),
            kc=epi_qk(1, kT_d, lambda i, tl, c0, nh: kT_d[c0 // 128:c0 // 128 + nh, :, tl * 128:(tl + 1) * 128].rearrange("h p t -> p h t"), False),
            v=epi_v,
            hg=epi_store(hg_d, lambda i, tl, c0, cw: hg_d[i, :, c0:c0 + cw], False),
            ffw=epi_store(fr_d, lambda i, tl, c0, cw: fr_d[tl, :, c0:c0 + cw], False),
            fbw=epi_store(fr_d, lambda i, tl, c0, cw: fr_d[tl, :, HW + c0:HW + c0 + cw], False),
            hi=epi_store(vi_d, lambda i, tl, c0, cw: vi_d[tl, :, c0:c0 + cw], True),
            hq=epi_hq,
        )
        wsrc = w_in_f[:, :].rearrange("(k p) n -> p k n", p=128)
        nblk = 0
        for (name, col0, ncols) in segs:
            epi = epis[name]
            for c0 in range(0, ncols, 512):
                cw = min(512, ncols - c0)
                w = wb[nblk % 2]; nblk += 1
                for k0 in range(0, KC, 8):
                    P.dma("sp", w, w[:, k0:k0 + 8, 0:cw], w_in_f, wsrc[:, k0:k0 + 8, col0 + c0:col0 + c0 + cw])
                for i, tl in enumerate(tiles):
                    ps = pg[cnt["b"] % 4] if False else pg[(nblk * G + i) % 4]
                    P.mm(ps, [dict(out=ps[:, 0:cw], lhsT=hts[i][:, k, :], rhs=w[:, k, 0:cw], start=(k == 0), stop=(k == KC - 1))
                              for k in range(KC)], [hts[i], w])
                    epi(i, tl, c0, cw, ps)

    own_tiles = list(range(TO, 2 * TO)); oth_tiles = list(range(0, TO)); ctx_tiles = list(range(2 * TO, NT))
    mk = P.mark()
    stage_inproj("ipo", own_tiles, [("q", cm["q"], AW), ("hq", cm["hq"], HW), ("hg", cm["hg"], HW), ("k", cm["k"], KVW),
                                    ("v", cm["v"], KVW), ("ffw", cm["ffw"], HW), ("fbw", cm["fbw"], HW), ("hi", cm["hi"], HW)], TO)
    P.barrier(); P.release(mk); mk = P.mark()
    stage_inproj("ipx", oth_tiles, [("k", cm["k"], KVW), ("v", cm["v"], KVW), ("ffw", cm["ffw"], HW),
                                    ("fbw", cm["fbw"], HW), ("hi", cm["hi"], HW)], 0)
    P.barrier(); P.release(mk); mk = P.mark()
    stage_inproj("ipc", ctx_tiles, [("kc", cm["k"], KVW), ("v", cm["v"], KVW), ("ffw", cm["ffw"], HW),
                                    ("fbw", cm["fbw"], HW), ("hi", cm["hi"], HW)], None)
    P.barrier(); P.release(mk)
    for nm, t_, shp, dt_ in (("qT", qT_d, [AH, 128, HALF], BF16), ("kT", kT_d, [KVH, 128, cfg.NTOK], BF16),
                             ("v", v_d, [NT, 128, KVH * 129], BF16), ("fr", fr_d, [NT, 128, 2 * HW], F32),
                             ("vi", vi_d, [NT, 128, HW], BF16), ("hg", hg_d, [TO, 128, HW], F32), ("qh", qh_d, [HH, 128, HALF], F32)):
        d = dbg_out(nm, shp, dt_)
        if d is not None:
            P.dma("sp", d, d[:, :, :], t_, t_[:, :, :])
            P.barrier()
    if stop_after <= 4:
        return finish(nc, P, io, out, dbg)

    def stage_attn():
        NTK = NT
        QB = min(512, HALF)
        kT = P.sb("at_kT", [128, KVH, cfg.NTOK], BF16)
        for g in range(KVH):
            P.dma("sp", kT, kT[:, g, :], kT_d, kT_d[g, :, :])
        vv = P.sb("at_v", [128, NTK, KVH * 129], BF16)
        for t in range(NTK):
            P.dma("sp", vv, vv[:, t, :], v_d, v_d[t, :, :])
        qs = [P.sb(f"at_q{i}", [128, QB], BF16) for i in range(2)]
        pts = [P.sb(f"at_pt{i}", [128, NTK, QB], BF16) for i in range(2)]
        pss = [P.ps(f"at_ps{i}", [128, 512]) for i in range(4)]
        pso = [P.ps(f"at_po{i}", [128, 512]) for i in range(2)]
        rcp = [P.sb(f"at_rc{i}", [128, 1], F32) for i in range(2)]
        ost = [P.sb(f"at_o{i}", [128, AW], F32) for i in range(QB // 128)]
        scale = 128 ** -0.5
        n = 0
        for qb in range(HALF // QB):
            for h in range(AH):
                g = h // 4
                q = qs[n % 2]; pt_ = pts[n % 2]; n += 1
                P.dma("sp", q, q[:], qT_d, qT_d[h, :, qb * QB:(qb + 1) * QB])
                for kc in range(NTK):
                    ps = pss[kc % 4]
                    P.mm(ps, [dict(out=ps[:, 0:QB], lhsT=kT[:, g, kc * 128:(kc + 1) * 128], rhs=q[:, :], start=True, stop=True)], [kT, q])
                    P.op("act", lambda e, ps=ps, kc=kc, pt_=pt_: e.activation(out=pt_[:, kc, :], in_=ps[:, 0:QB], func=ACT.Exp, scale=scale), [ps], [pt_])
                for s_ in range(QB // 128):
                    po = pso[s_ % 2]; rc_ = rcp[s_ % 2]
                    P.mm(po, [dict(out=po[:, 0:129], lhsT=pt_[:, kc, s_ * 128:(s_ + 1) * 128], rhs=vv[:, kc, g * 129:(g + 1) * 129],
                                   start=(kc == 0), stop=(kc == NTK - 1)) for kc in range(NTK)], [pt_, vv])
                    P.op("dve", lambda e, po=po, rc_=rc_: e.reciprocal(out=rc_[:], in_=po[:, 128:129]), [po], [rc_])
                    o = ost[s_]
                    P.op("dve", lambda e, po=po, rc_=rc_, o=o, h=h: e.tensor_scalar(out=o[:, h * 128:(h + 1) * 128], in0=po[:, 0:128], scalar1=rc_[:, 0:1],
                                                                                     scalar2=None, op0=ALU.mult), [po, rc_], [o])
            for s_ in range(QB // 128):
                tix = qb * (QB // 128) + s_
                P.dma("sp", mix_d, mix_d[tix, :, 0:AW], ost[s_], ost[s_][:])

    mk = P.mark()
    stage_attn()
    P.barrier(); P.release(mk)
    d = dbg_out("mix", [TO, 128, MIXW], F32)
    if d is not None:
        P.dma("sp", d, d[:, :, :], mix_d, mix_d[:, :, :])
        P.barrier()
    if stop_after <= 5:
        return finish(nc, P, io, out, dbg)

    def stage_hgrn():
        C2 = HH * 64
        lb = P.sb("hg_lb", [64, 2 * HW], F32)
        oml = P.sb("hg_oml", [64, 2 * HW], F32)
        P.dma("sp", lb, lb[:], io.lbT, io.lbT[0:1, 0:2 * HW].partition_broadcast(64))
        P.dma("sp", oml, oml[:], io.lbT, io.lbT[0:1, 2 * HW:4 * HW].partition_broadcast(64))
        P.op("dve", lambda e: e.tensor_tensor(out=lb[:], in0=lb[:], in1=oml[:], op=ALU.subtract), [lb, oml], [lb])
        P.op("act", lambda e: e.activation(out=lb[:], in_=lb[:], func=ACT.Sigmoid), [lb], [lb])
        P.op("dve", lambda e: e.tensor_scalar(out=oml[:], in0=lb[:], scalar1=-1.0, scalar2=1.0, op0=ALU.mult, op1=ALU.add), [lb], [oml])
        fl = P.sb("hg_fl", [64, 2], F32)
        P.dma("sp", fl, fl[:], io.flags, io.flags[0:1, :].partition_broadcast(64))
        hm = P.sb("hg_hm", [64, 6, 64], F32)
        P.dma("sp", hm, hm[:], io.hmats, io.hmats[:, :].rearrange("p (a c) -> p a c", c=64))
        maskH = P.sb("hg_mask", [64, 2, HH, 64], F32)
        for d_ in range(2):
            for h in range(HH):
                P.op("dve", lambda e, d_=d_, h=h: e.tensor_copy(out=maskH[:, d_, h, :], in_=hm[:, 3 * d_ + 1, :]), [hm], [maskH])
        hgn = P.sb("hg_gain", [64, 128], F32)
        P.dma("sp", hgn, hgn[:], io.hg_gain, io.hg_gain[0:1, :].partition_broadcast(64))
        S = P.sb("hg_S", [128, HH, 128], F32)
        Sb = P.sb("hg_Sb", [128, HH, 128], BF16)
        X = P.ps("hg_X", [128, HW])
        Y = P.ps("hg_Y", [128, HW])
        nb2 = 2
        fraw = [P.sb(f"hg_fraw{i}", [64, HW], F32) for i in range(nb2)]
        vch = [P.sb(f"hg_v{i}", [64, HW], BF16) for i in range(nb2)]
        qch = [P.sb(f"hg_q{i}", [128, HH, 64], F32) for i in range(nb2)]
        gch = [P.sb(f"hg_g{i}", [64, HW], F32) for i in range(1)]
        ofc = [P.sb(f"hg_of{i}", [64, HW], F32) for i in range(1)]
        fw_ = P.sb("hg_f", [64, HW], F32)
        LF = P.sb("hg_LF", [64, HW], F32)
        Kt = P.sb("hg_K", [64, HW], F32)
        ek = P.sb("hg_ek", [64, HW], F32)
        khat = P.sb("hg_khat", [64, HW], BF16)
        X1 = P.sb("hg_X1", [128, HH, 64], F32)
        X2 = P.sb("hg_X2", [128, HH, 64], F32)
        X3 = P.sb("hg_X3", [128, HH, 64], F32)
        dec = P.sb("hg_dec", [128, HH], F32)
        ktil = P.sb("hg_ktil", [128, HH, 64], BF16)
        qtil = P.sb("hg_qtil", [128, HH, 64], BF16)
        qe = P.sb("hg_qe", [128, HH, 64], BF16)
        AT = P.sb("hg_AT", [64, HH, 64], BF16)
        osb = P.sb("hg_os", [64, HW], F32)
        sqb = P.sb("hg_sq", [64, HW], F32)
        ssr = P.sb("hg_ssr", [64, HH], F32)
        onb = P.sb("hg_on", [64, HW], F32)
        sgb = sqb
        nchunk = {"n": 0}

        def chunk(d_, kind, tl, half, own_i, last_pass):
            n = nchunk["n"]; nchunk["n"] += 1
            k = n % nb2
            r0 = half * 64
            fr_, v_ = fraw[k], vch[k]
            P.dma("sp", fr_, fr_[:], fr_d, fr_d[tl, r0:r0 + 64, d_ * HW:(d_ + 1) * HW])
            P.dma("sp", v_, v_[:], vi_d, vi_d[tl, r0:r0 + 64, :])
            own = kind == "own"
            if own:
                q_ = qch[k]
                t0 = own_i * 128 + r0
                P.dma("sp", q_, q_[:], qh_d, qh_d[:, :, t0:t0 + 64].rearrange("h p t -> p h t"))
                if last_pass:
                    g_, of_ = gch[0], ofc[0]
                    P.dma("sp", g_, g_[:], hg_d, hg_d[own_i, r0:r0 + 64, :])
                    P.dma("sp", of_, of_[:], of_d, of_d[t0:t0 + 64, :])
            P.op("act", lambda e: e.activation(out=fw_[:], in_=fr_[:], func=ACT.Sigmoid), [fr_], [fw_])
            P.op("dve", lambda e: e.tensor_tensor(out=fw_[:], in0=fw_[:], in1=oml[:, d_ * HW:(d_ + 1) * HW], op=ALU.mult), [fw_, oml], [fw_])
            P.op("dve", lambda e: e.tensor_tensor(out=fw_[:], in0=fw_[:], in1=lb[:, d_ * HW:(d_ + 1) * HW], op=ALU.add), [fw_, lb], [fw_])
            P.op("act", lambda e: e.activation(out=LF[:], in_=fw_[:], func=ACT.Ln), [fw_], [LF])
            P.op("dve", lambda e: e.tensor_scalar(out=Kt[:], in0=fw_[:], scalar1=-1.0, scalar2=1.0, op0=ALU.mult, op1=ALU.add), [fw_], [Kt])
            if kind == "oth":
                P.op("dve", lambda e: e.tensor_scalar(out=LF[:], in0=LF[:], scalar1=fl[:, d_:d_ + 1], scalar2=None, op0=ALU.mult), [LF, fl], [LF])
                P.op("dve", lambda e: e.tensor_scalar(out=Kt[:], in0=Kt[:], scalar1=fl[:, d_:d_ + 1], scalar2=None, op0=ALU.mult), [Kt, fl], [Kt])
            P.mm(X, [dict(out=X[0:64, c:c + min(512, HW - c)], lhsT=hm[:, 3 * d_ + 2, :], rhs=LF[:, c:c + min(512, HW - c)], start=True, stop=True)
                     for c in range(0, HW, 512)], [hm, LF])
            P.op("act", lambda e: e.activation(out=ek[:], in_=X[0:64, 0:HW], func=ACT.Exp), [X], [ek])
            P.op("dve", lambda e: e.tensor_tensor(out=khat[:], in0=ek[:], in1=Kt[:], op=ALU.mult), [ek, Kt], [khat])
            Yv = Y[:, :].rearrange("p (h c) -> p h c", c=128)
            P.mm(Y, [dict(out=Yv[:, h, :], lhsT=LF[:, h * 128:(h + 1) * 128], rhs=hm[:, 3 * d_:3 * d_ + 2, :].rearrange("p a c -> p (a c)"),
                          start=True, stop=True) for h in range(HH)], [LF, hm])
            last = 63 if d_ == 0 else 0
            if own:
                P.op("act", lambda e: e.activation(out=X1[:], in_=Yv[:, :, 0:64], func=ACT.Exp), [Y], [X1])
                P.op("act", lambda e: e.activation(out=X2[:], in_=Yv[:, :, 0:64], func=ACT.Exp, scale=-1.0), [Y], [X2])
                P.op("act", lambda e: e.activation(out=X3[:], in_=Yv[:, :, 64:128], func=ACT.Exp), [Y], [X3])
                P.op("dve", lambda e: e.tensor_copy(out=dec[:], in_=X3[:, :, last]), [X3], [dec])
                Xk = X[:, 0:C2].rearrange("p (h c) -> p h c", c=64)
                P.mm(X, [dict(out=Xk[:, h, :], lhsT=Kt[:, h * 128:(h + 1) * 128], rhs=ident[0:64, 0:64], start=True, stop=True)
                         for h in range(HH)], [Kt, ident])
                P.op("dve", lambda e: e.tensor_tensor(out=ktil[:], in0=Xk, in1=X2[:], op=ALU.mult), [X, X2], [ktil])
                P.op("dve", lambda e: e.tensor_tensor(out=qtil[:], in0=q_[:], in1=X1[:], op=ALU.mult), [q_, X1], [qtil])
                P.op("dve", lambda e: e.tensor_tensor(out=qe[:], in0=q_[:], in1=X3[:], op=ALU.mult), [q_, X3], [qe])
                Xa = X[0:64, C2:2 * C2].rearrange("p (h c) -> p h c", c=64) if 2 * C2 <= HW else None
                P.mm(X, [dict(out=Xa[:, h, :], lhsT=ktil[:, h, :], rhs=qtil[:, h, :], start=True, stop=True) for h in range(HH)], [ktil, qtil])
                P.op("dve", lambda e: e.tensor_tensor(out=AT[:], in0=Xa, in1=maskH[:, d_, :, :], op=ALU.mult), [X, maskH], [AT])
            else:
                P.op("act", lambda e: e.activation(out=dec[:], in_=Yv[:, :, 64 + last], func=ACT.Exp), [Y], [dec])
            Yu = Y[:, :].rearrange("p (h c) -> p h c", c=128)
            P.mm(Y, [dict(out=Yu[:, h, :], lhsT=khat[:, h * 128:(h + 1) * 128], rhs=v_[:, h * 128:(h + 1) * 128], start=True, stop=True)
                     for h in range(HH)], [khat, v_])
            if own:
                Xo = X[0:64, 0:HW].rearrange("p (h c) -> p h c", c=128)
                items = []
                for h in range(HH):
                    items.append(dict(out=Xo[:, h, :], lhsT=AT[:, h, :], rhs=v_[:, h * 128:(h + 1) * 128], start=True, stop=False))
                    items.append(dict(out=Xo[:, h, :], lhsT=qe[:, h, :], rhs=Sb[:, h, :], start=False, stop=True))
                P.mm(X, items, [AT, v_, qe, Sb])
            for h in range(HH):
                P.op("dve", lambda e, h=h: e.scalar_tensor_tensor(out=S[:, h, :], in0=S[:, h, :], scalar=dec[:, h:h + 1], in1=Yu[:, h, :],
                                                                   op0=ALU.mult, op1=ALU.add), [S, dec, Y], [S])
            P.op("act", lambda e: e.copy(out=Sb[:], in_=S[:]), [S], [Sb])
            if own:
                Xf = X[0:64, 0:HW]
                if not last_pass:
                    P.op("dve", lambda e: e.tensor_copy(out=osb[:], in_=Xf), [X], [osb])
                    P.dma("sp", of_d, of_d[t0:t0 + 64, :], osb, osb[:])
                else:
                    P.op("dve", lambda e: e.tensor_tensor(out=osb[:], in0=Xf, in1=of_[:], op=ALU.add), [X, of_], [osb])
                    P.op("dve", lambda e: e.tensor_tensor(out=sqb[:], in0=osb[:], in1=osb[:], op=ALU.mult), [osb], [sqb])
                    P.op("dve", lambda e: e.tensor_reduce(out=ssr[:], in_=sqb[:].rearrange("p (h d) -> p h d", d=128), axis=AX.X, op=ALU.add), [sqb], [ssr])
                    P.op("act", lambda e: e.activation(out=ssr[:], in_=ssr[:], func=ACT.Sqrt, scale=1.0 / 128, bias=epsc[0:64, 0:1]), [ssr, epsc], [ssr])
                    P.op("dve", lambda e: e.reciprocal(out=ssr[:], in_=ssr[:]), [ssr], [ssr])
                    for h in range(HH):
                        hs = slice(h * 128, (h + 1) * 128)
                        P.op("dve", lambda e, h=h, hs=hs: e.scalar_tensor_tensor(out=onb[:, hs], in0=osb[:, hs], scalar=ssr[:, h:h + 1], in1=hgn[:, :],
                                                                                   op0=ALU.mult, op1=ALU.mult), [osb, ssr, hgn], [onb])
                    P.op("act", lambda e: e.activation(out=sgb[:], in_=g_[:], func=ACT.Silu), [g_], [sgb])
                    P.op("dve", lambda e: e.tensor_tensor(out=onb[:], in0=onb[:], in1=sgb[:], op=ALU.mult), [onb, sgb], [onb])
                    P.dma("sp", mix_d, mix_d[own_i, r0:r0 + 64, AW:AW + HW], onb, onb[:])

        for d_ in range(2):
            P.op("dve", lambda e: e.memset(S[:], 0.0), [], [S])
            P.op("dve", lambda e: e.memset(Sb[:], 0.0), [], [Sb])
            seq = []
            for tl in ctx_tiles:
                seq += [("ctx", tl, 0, None), ("ctx", tl, 1, None)]
            for tl in oth_tiles:
                seq += [("oth", tl, 0, None), ("oth", tl, 1, None)]
            for i, tl in enumerate(own_tiles):
                seq += [("own", tl, 0, i), ("own", tl, 1, i)]
            if d_ == 1:
                seq = ([s_ for s_ in seq if s_[0] == "ctx"][::-1] + [s_ for s_ in seq if s_[0] == "oth"][::-1]
                       + [s_ for s_ in seq if s_[0] == "own"][::-1])
            for (kind, tl, half, oi) in seq:
                chunk(d_, kind, tl, half, oi, d_ == 1)

    mk = P.mark()
    stage_hgrn()
    P.barrier(); P.release(mk)
    d = dbg_out("mix7", [TO, 128, MIXW], F32)
    if d is not None:
        P.dma("sp", d, d[:, :, :], mix_d, mix_d[:, :, :])
        P.barrier()
    if stop_after <= 7:
        return finish(nc, P, io, out, dbg)

    mk = P.mark()
    norm_transpose("n3", mix_d, lambda t: mix_d[t, :, :], TO, None, None, hT_d, 0, do_norm=False, W=MIXW)
    P.barrier(); P.release(mk)

    def stage_outproj():
        KM = MIXW // 128
        hts = []
        for i in range(TO):
            h = P.sb(f"op_hT{i}", [128, KM, 128], BF16)
            P.dma("sp", h, h[:], hT_d, hT_d[i, :, :].rearrange("p (k t) -> p k t", t=128))
            hts.append(h)
        wb = [P.sb(f"op_w{i}", [128, KM, 512], BF16) for i in range(2)]
        pg = [P.ps(f"op_pg{i}", [128, 512]) for i in range(4)]
        ob = [P.sb(f"op_o{i}", [128, 512], F32) for i in range(3)]
        wsrc = w_out_f[:, :].rearrange("(k p) n -> p k n", p=128)
        n = 0
        for c0 in range(0, D, 512):
            w = wb[(c0 // 512) % 2]
            for k0 in range(0, KM, 8):
                P.dma("sp", w, w[:, k0:k0 + 8, :], w_out_f, wsrc[:, k0:k0 + 8, c0:c0 + 512])
            for i in range(TO):
                ps = pg[n % 4]; o = ob[n % 3]; n += 1
                P.mm(ps, [dict(out=ps[:, :], lhsT=hts[i][:, k, :], rhs=w[:, k, :], start=(k == 0), stop=(k == KM - 1)) for k in range(KM)], [hts[i], w])
                P.op("act" if n % 2 else "dve", (lambda e, o=o, ps=ps: e.copy(out=o[:], in_=ps[:])) if n % 2 else
                     (lambda e, o=o, ps=ps: e.tensor_copy(out=o[:], in_=ps[:])), [ps], [o])
                P.dma("sp", y_d, y_d[i, :, c0:c0 + 512], o, o[:])

    mk = P.mark()
    stage_outproj()
    P.barrier(); P.release(mk)

    def rms_residual(tag, y_ap, x_ap, y_t, x_t, gi, dst_t, dst_ap, n_tiles):
        gb = P.sb(f"{tag}_gb", [128, D], F32)
        P.dma("sp", gb, gb[:], g_d, g_d[0:1, gi * D:(gi + 1) * D].partition_broadcast(128))
        yt = [P.sb(f"{tag}_y{i}", [128, D], F32) for i in range(2)]
        xt = [P.sb(f"{tag}_x{i}", [128, D], F32) for i in range(2)]
        junk = P.sb(f"{tag}_junk", [128, D], BF16)
        ss = [P.sb(f"{tag}_ss{i}", [128, 2], F32) for i in range(2)]
        for t in range(n_tiles):
            y = yt[t % 2]; x = xt[t % 2]; s = ss[t % 2]
            if y_t is not None:
                P.dma("sp", y, y[:], y_t, y_ap(t))
            else:
                y = y_ap(t)
            P.dma("sp", x, x[:], x_t, x_ap(t))
            P.op("dve", lambda e, s=s: e.memset(s[:], 0.0), [], [s])
            P.op("act", lambda e, y=y, s=s: e.activation(out=junk[:], in_=y[:], func=ACT.Square, accum_out=s[:, 0:1]), [y, s], [junk, s])
            P.op("act", lambda e, s=s: e.activation(out=s[:, 1:2], in_=s[:, 0:1], func=ACT.Sqrt, scale=1.0 / D, bias=epsc[:, 0:1]), [s, epsc], [s])
            P.op("dve", lambda e, s=s: e.reciprocal(out=s[:, 1:2], in_=s[:, 1:2]), [s], [s])
            P.op("dve", lambda e, y=y, s=s: e.scalar_tensor_tensor(out=y[:], in0=y[:], scalar=s[:, 1:2], in1=gb[:], op0=ALU.mult, op1=ALU.mult), [y, s, gb], [y])
            P.op("dve", lambda e, y=y, x=x: e.tensor_tensor(out=x[:], in0=x[:], in1=y[:], op=ALU.add), [x, y], [x])
            P.dma("sp", dst_t, dst_ap(t), x, x[:])

    mk = P.mark()
    rms_residual("r1", lambda t: y_d[t, :, :], lambda t: io.x_own[t * 128:(t + 1) * 128, :], y_d, io.x_own, 0, x1_d, lambda t: x1_d[t, :, :], TO)
    P.barrier(); P.release(mk)
    d = dbg_out("x1", [TO, 128, D], F32)
    if d is not None:
        P.dma("sp", d, d[:, :, :], x1_d, x1_d[:, :, :])
        P.barrier()

    mk = P.mark()
    wrt = P.sb("rt_w", [128, KC, NE], F32)
    P.dma("sp", wrt, wrt[:], io.w_rt, io.w_rt[:, :].rearrange("p (k e) -> p k e", e=NE))
    brt = P.sb("rt_b", [128, NE], F32)
    P.dma("sp", brt, brt[:], io.b_rt, io.b_rt[0:1, :].partition_broadcast(128))
    h2f = [P.sb(f"rt_h{i}", [128, 4, 128], F32) for i in range(2)]
    prt = P.ps("rt_ps", [128, NE])
    lg = P.sb("rt_lg", [128, NE], F32)
    mx8 = P.sb("rt_mx", [128, 8], F32)
    nmx = P.sb("rt_nmx", [128, 1], F32)
    msk = P.sb("rt_msk", [128, NE], F32)
    exs = P.sb("rt_ex", [128, NE], F32)
    sm = P.sb("rt_sm", [128, 1], F32)
    hk = {"n": 0}

    def router_hook(t, c0, n, ps):
        hf_ = h2f[hk["n"] % 2]; hk["n"] += 1
        for i in range(n):
            c = c0 + i
            P.op("dve", lambda e, i=i, c=c: e.tensor_scalar(out=hf_[:, i, :], in0=ps[:, i * 128:(i + 1) * 128], scalar1=coef[:, 4, c:c + 1],
                                                            scalar2=coef[:, 5, c:c + 1], op0=ALU.mult, op1=ALU.add), [ps, coef], [hf_])
        P.mm(prt, [dict(out=prt[:, :], lhsT=hf_[:, i, :], rhs=wrt[:, c0 + i, :], start=(c0 + i == 0), stop=(c0 + i == KC - 1)) for i in range(n)],
             [hf_, wrt])
        if c0 + n == KC:
            P.op("dve", lambda e: e.tensor_tensor(out=lg[:], in0=prt[:, :], in1=brt[:], op=ALU.add), [prt, brt], [lg])
            P.op("dve", lambda e: e.max(out=mx8[:], in_=lg[:]), [lg], [mx8])
            P.op("dve", lambda e: e.tensor_scalar(out=nmx[:], in0=mx8[:, 0:1], scalar1=-1.0, scalar2=None, op0=ALU.mult), [mx8], [nmx])
            P.op("dve", lambda e: e.tensor_scalar(out=msk[:], in0=lg[:], scalar1=mx8[:, cfg.TOPK - 1:cfg.TOPK], scalar2=None, op0=ALU.is_ge), [lg, mx8], [msk])
            P.op("act", lambda e: e.activation(out=exs[:], in_=lg[:], func=ACT.Exp, bias=nmx[:, 0:1]), [lg, nmx], [exs])
            P.op("dve", lambda e: e.tensor_tensor(out=exs[:], in0=exs[:], in1=msk[:], op=ALU.mult), [exs, msk], [exs])
            P.op("dve", lambda e: e.tensor_reduce(out=sm[:], in_=exs[:], axis=AX.X, op=ALU.add), [exs], [sm])
            P.op("dve", lambda e: e.reciprocal(out=sm[:], in_=sm[:]), [sm], [sm])
            P.op("dve", lambda e, t=t: e.tensor_scalar(out=cmb[:, t, :], in0=exs[:], scalar1=sm[:, 0:1], scalar2=None, op0=ALU.mult), [exs, sm], [cmb])

    norm_transpose("n2", x1_d, lambda t: x1_d[t, :, :], TO, 4, 5, hT_d, TO, hook=router_hook)
    P.barrier(); P.release(mk)
    d = dbg_out("cmb", [128, TO * NE], F32)
    if d is not None:
        P.dma("sp", d, d[:, :], cmb, cmb[:].rearrange("p t e -> p (t e)"))
        P.barrier()
    if stop_after <= 8:
        return finish(nc, P, io, out, dbg)

    def stage_moe():
        TP = min(512, HALF)
        TPT = TP // 128
        bg = P.sb("me_bg", [128, NE * JE], F32); P.dma("sp", bg, bg[:], io.bgT, io.bgT[:, :])
        bu = P.sb("me_bu", [128, NE * JE], F32); P.dma("sp", bu, bu[:], io.buT, io.buT[:, :])
        bdn = [P.sb(f"me_bdn{i}", [NE, 512], F32) for i in range(2)]
        gbb = [P.sb(f"me_gb{i}", [128, 512], F32) for i in range(2)]
        xbb = [P.sb(f"me_xb{i}", [128, 512], F32) for i in range(2)]
        h2 = P.sb("me_h2", [128, KC, TP], BF16)
        acc = [P.sb(f"me_acc{i}", [128, D], F32) for i in range(TPT)]
        wgb = [P.sb(f"me_wg{i}", [128, KC, 128], BF16) for i in range(2)]
        wub = [P.sb(f"me_wu{i}", [128, KC, 128], BF16) for i in range(2)]
        wdb = [P.sb(f"me_wd{i}", [128, JE, 512], BF16) for i in range(2)]
        actT = [P.sb(f"me_act{i}", [128, JE, TP], BF16) for i in range(1)]
        gc = [P.sb(f"me_gc{i}", [128, TP], F32) for i in range(2)]
        uc = [P.sb(f"me_uc{i}", [128, TP], F32) for i in range(2)]
        sg = [P.sb(f"me_sg{i}", [128, TP], F32) for i in range(2)]
        cT = P.sb("me_cT", [NE, TPT, 128], F32)
        psg = [P.ps(f"me_pg{i}", [128, 512]) for i in range(2)]
        psu = [P.ps(f"me_pu{i}", [128, 512]) for i in range(2)]
        psd = [P.ps(f"me_pd{i}", [128, 512]) for i in range(4)]
        ss = P.sb("me_ss", [128, 2], F32)
        nd = 0
        for ps_ in range(HALF // TP):
            for tt in range(TPT):
                P.dma("sp", h2, h2[:, :, tt * 128:(tt + 1) * 128], hT_d, hT_d[TO + ps_ * TPT + tt, :, :].rearrange("p (k t) -> p k t", t=128))
            pc = psd[0]
            P.mm(pc, [dict(out=pc[0:NE, tt * 128:(tt + 1) * 128], lhsT=cmb[:, ps_ * TPT + tt, :], rhs=ident[:, :], start=True, stop=True)
                      for tt in range(TPT)], [cmb, ident])
            P.op("dve", lambda e: e.tensor_copy(out=cT[:], in_=pc[0:NE, 0:TP].rearrange("p (t c) -> p t c", c=128)), [pc], [cT])
            for nb in range(NB):
                bd = bdn[nb % 2]
                P.dma("sp", bd, bd[:], io.b_dn, io.b_dn[:, nb * 512:(nb + 1) * 512])
                for tt in range(TPT):
                    pd = psd[nd % 4]; nd += 1
                    P.mm(pd, [dict(out=pd[:, :], lhsT=cT[:, tt, :], rhs=bd[:, :], start=True, stop=True)], [cT, bd])
                    P.op("act", lambda e, pd=pd, tt=tt, nb=nb: e.copy(out=acc[tt][:, nb * 512:(nb + 1) * 512], in_=pd[:, :]), [pd], [acc[tt]])
            ne = 0
            for i_ in range(EL):
                for r_ in range(NCORES):
                    e_ = r_ * EL + i_
                    at = actT[0]; ne += 1
                    for j in range(JE):
                        k2 = (ne * JE + j) % 2
                        wg_, wu_ = wgb[k2], wub[k2]
                        row0 = r_ * RJ + j * 128
                        P.dma("sp", wg_, wg_[:], eg_f[i_], eg_f[i_][row0:row0 + 128, :].rearrange("p (k c) -> p k c", c=128))
                        P.dma("sp", wu_, wu_[:], eu_f[i_], eu_f[i_][row0:row0 + 128, :].rearrange("p (k c) -> p k c", c=128))
                        pg_, pu_ = psg[k2], psu[k2]
                        P.mm(pg_, [dict(out=pg_[:, 0:TP], lhsT=wg_[:, k, :], rhs=h2[:, k, :], start=(k == 0), stop=(k == KC - 1)) for k in range(KC)], [wg_, h2])
                        P.mm(pu_, [dict(out=pu_[:, 0:TP], lhsT=wu_[:, k, :], rhs=h2[:, k, :], start=(k == 0), stop=(k == KC - 1)) for k in range(KC)], [wu_, h2])
                        col = e_ * JE + j
                        g_, u_, s_ = gc[k2], uc[k2], sg[k2]
                        P.op("dve", lambda e, g_=g_, pg_=pg_, col=col: e.tensor_scalar(out=g_[:], in0=pg_[:, 0:TP], scalar1=bg[:, col:col + 1], scalar2=7.0,
                                                                                       op0=ALU.add, op1=ALU.min), [pg_, bg], [g_])
                        P.op("act", lambda e, g_=g_, s_=s_: e.activation(out=s_[:], in_=g_[:], func=ACT.Sigmoid, scale=1.702), [g_], [s_])
                        P.op("dve", lambda e, u_=u_, pu_=pu_, col=col: e.tensor_scalar(out=u_[:], in0=pu_[:, 0:TP], scalar1=bu[:, col:col + 1], scalar2=7.0,
                                                                                       op0=ALU.add, op1=ALU.min), [pu_, bu], [u_])
                        P.op("dve", lambda e, u_=u_: e.tensor_scalar(out=u_[:], in0=u_[:], scalar1=-7.0, scalar2=1.0, op0=ALU.max, op1=ALU.add), [u_], [u_])
                        P.op("dve", lambda e, g_=g_, s_=s_: e.tensor_tensor(out=g_[:], in0=g_[:], in1=s_[:], op=ALU.mult), [g_, s_], [g_])
                        P.op("dve", lambda e, g_=g_, u_=u_, at=at, j=j: e.tensor_tensor(out=at[:, j, :], in0=g_[:], in1=u_[:], op=ALU.mult), [g_, u_], [at])
                    for nb in range(NB):
                        wd_ = wdb[(ne * NB + nb) % 2]
                        wv = wd_view(ed_f[i_], r_ * RJ)
                        P.dma("sp", wd_, wd_[:], ed_f[i_], wv[nb * 128:(nb + 1) * 128, :].rearrange("p (j c) -> p j c", c=512))
                        for tt in range(TPT):
                            pd = psd[nd % 4]; nd += 1
                            P.mm(pd, [dict(out=pd[:, :], lhsT=at[:, j, tt * 128:(tt + 1) * 128], rhs=wd_[:, j, :], start=(j == 0), stop=(j == JE - 1))
                                      for j in range(JE)], [at, wd_])
                            a_ = acc[tt]
                            P.op("dve", lambda e, pd=pd, a_=a_, nb=nb, tt=tt, e_=e_, ps_=ps_: e.scalar_tensor_tensor(
                                out=a_[:, nb * 512:(nb + 1) * 512], in0=pd[:, :], scalar=cmb[:, ps_ * TPT + tt, e_:e_ + 1],
                                in1=a_[:, nb * 512:(nb + 1) * 512], op0=ALU.mult, op1=ALU.add), [pd, cmb, a_], [a_])
            h2flat = h2[:, :, :].rearrange("p k t -> p (k t)")
            nf = 0
            for tt in range(TPT):
                tix = ps_ * TPT + tt
                a_ = acc[tt]
                P.op("dve", lambda e: e.memset(ss[:], 0.0), [], [ss])
                P.op("act", lambda e, a_=a_: e.activation(out=h2flat[:, 0:D], in_=a_[:], func=ACT.Square, accum_out=ss[:, 0:1]), [a_, ss], [h2, ss])
                P.op("act", lambda e: e.activation(out=ss[:, 1:2], in_=ss[:, 0:1], func=ACT.Sqrt, scale=1.0 / D, bias=epsc[:, 0:1]), [ss, epsc], [ss])
                P.op("dve", lambda e: e.reciprocal(out=ss[:, 1:2], in_=ss[:, 1:2]), [ss], [ss])
                for nb in range(NB):
                    gb_, xb_ = gbb[nf % 2], xbb[nf % 2]; nf += 1
                    cs = slice(nb * 512, (nb + 1) * 512)
                    P.dma("sp", gb_, gb_[:], g_d, g_d[0:1, D + nb * 512:D + (nb + 1) * 512].partition_broadcast(128))
                    P.dma("sp", xb_, xb_[:], x1_d, x1_d[tix, :, cs])
                    P.op("dve", lambda e, a_=a_, cs=cs, gb_=gb_: e.scalar_tensor_tensor(out=a_[:, cs], in0=a_[:, cs], scalar=ss[:, 1:2], in1=gb_[:],
                                                                                        op0=ALU.mult, op1=ALU.mult), [a_, ss, gb_], [a_])
                    P.op("dve", lambda e, a_=a_, cs=cs, xb_=xb_: e.tensor_tensor(out=a_[:, cs], in0=a_[:, cs], in1=xb_[:], op=ALU.add), [a_, xb_], [a_])
                P.dma("sp", out, out[tix * 128:(tix + 1) * 128, :], a_, a_[:])

    mk = P.mark()
    stage_moe()
    P.barrier(); P.release(mk)
    return finish(nc, P, io, out, dbg)


def finish(nc, P, io, out, dbg):
    P.final_wait("pool")
    P.final_wait("sp")
    P.emit()
    P.close()
    return nc


def static_tables(cfg):
    ident = np.eye(128, dtype=np.float32)
    s = np.arange(64)
    tri_f = (s[:, None] <= s[None, :]).astype(np.float32)
    tri_b = (s[:, None] >= s[None, :]).astype(np.float32)
    d_f = tri_f - (s[:, None] <= 31).astype(np.float32)
    d_b = tri_b - (s[:, None] >= 32).astype(np.float32)
    su_f = (s[:, None] > s[None, :]).astype(np.float32)
    su_b = (s[:, None] < s[None, :]).astype(np.float32)
    hmats = np.concatenate([d_f, tri_f, su_f, d_b, tri_b, su_b], axis=1).astype(np.float32)
    amask_f = (s[:, None] <= s[None, :]).astype(np.float32)
    return ident, hmats, amask_f


def rope_tables(cfg, pos):
    AXD = 64
    inv = (10000.0 ** (-np.arange(0, AXD, 2, dtype=np.float32) / AXD)).astype(np.float32)
    row = (pos // cfg.GRID_W).astype(np.float32)
    col = (pos % cfg.GRID_W).astype(np.float32)
    ar = row[:, None] * inv[None, :]
    ac = col[:, None] * inv[None, :]
    cr, sr, cc, sc = np.cos(ar), np.sin(ar), np.cos(ac), np.sin(ac)
    C = np.concatenate([cr, cr, cc, cc], axis=1).astype(np.float32)
    S = np.concatenate([-sr, sr, -sc, sc], axis=1).astype(np.float32)
    return C, S


def prep_inputs(cfg, inp):
    D, KC, HALF, NE, EL, JE, NMODC = cfg.D, cfg.KC, cfg.HALF, cfg.NE, cfg.EL, cfg.JE, cfg.NMODC
    NB = D // 512
    f = lambda a: np.ascontiguousarray(np.asarray(a, dtype=np.float32))
    x, c, ctx, c_ctx = f(inp["x"]), f(inp["c"]), f(inp["ctx"]), f(inp["c_ctx"])
    w_ada, b_ada, gains = f(inp["w_ada"])[0], f(inp["b_ada"])[0], f(inp["norm_gains"])[0]
    w_in, w_out = np.asarray(inp["w_in"])[0], np.asarray(inp["w_out"])[0]
    ident, hmats, amask = static_tables(cfg)
    c5 = np.concatenate([c, c_ctx[None]], axis=0)
    c5T = f(c5.reshape(5, KC, 128).transpose(2, 1, 0).reshape(128, KC * 5))
    gainsT = f(gains.reshape(4, KC, 128).transpose(2, 0, 1).reshape(128, 4 * KC))
    lbl = f(inp["hgrn_lb_logits"])
    lbT = f(lbl.reshape(1, -1))
    w_rt = f(np.asarray(inp["w_router"])[0].reshape(KC, 128, NE).transpose(1, 0, 2).reshape(128, KC * NE))
    b_rt = f(np.asarray(inp["b_router"])[0][None])
    bgT = f(np.asarray(inp["b_gate"])[0].reshape(NE, JE, 128).transpose(2, 0, 1).reshape(128, NE * JE))
    buT = f(np.asarray(inp["b_up"])[0].reshape(NE, JE, 128).transpose(2, 0, 1).reshape(128, NE * JE))
    b_dn = f(np.asarray(inp["b_down"])[0])
    qk_gain = f(np.stack([np.asarray(inp["q_norm_gain"])[0], np.asarray(inp["k_norm_gain"])[0]]))
    hg_gain = f(np.asarray(inp["hgrn_norm_gain"])[0][None])
    wg, wu, wd = np.asarray(inp["w_gate"])[0], np.asarray(inp["w_up"])[0], np.asarray(inp["w_down"])[0]
    maps = []
    for core in range(NCORES):
        b, hf = core // 2, core % 2
        own = slice(hf * HALF, (hf + 1) * HALF)
        oth = slice((1 - hf) * HALF, (2 - hf) * HALF)
        pos = np.concatenate([np.arange(oth.start, oth.stop), np.arange(own.start, own.stop)])
        C, S = rope_tables(cfg, pos)
        selm = np.zeros((40, 16), np.float32)
        for r in range(8):
            selm[r * 5 + b, r] = 1.0
            selm[r * 5 + 4, 8 + r] = 1.0
        es = slice(core * EL, (core + 1) * EL)
        m = {
            "x_oth": f(x[b, oth]), "x_own": f(x[b, own]), "ctx_b": f(ctx[b]),
            "c5T": c5T, "selm": selm,
            "w_ada_s": f(w_ada[:, core * NMODC:(core + 1) * NMODC]),
            "b_ada_s": f(b_ada[core * NMODC:(core + 1) * NMODC][None]),
            "gainsT": gainsT,
            "w_in_s": f(w_in[core * (D // 8):(core + 1) * (D // 8)]),
            "w_out_s": f(w_out[core * (cfg.MIXW // 8):(core + 1) * (cfg.MIXW // 8)]),
            "ropeC": C, "ropeS": S, "qk_gain": qk_gain, "hg_gain": hg_gain, "lbT": lbT,
            "flags": np.array([[float(hf), float(1 - hf)]], np.float32),
            "ident": ident, "hmats": hmats, "amask": amask, "w_rt": w_rt, "b_rt": b_rt,
            "wg_s": f(wg[es].reshape(EL, KC, 128, JE, 128).transpose(0, 3, 2, 1, 4).reshape(EL * JE * 128, D)),
            "wu_s": f(wu[es].reshape(EL, KC, 128, JE, 128).transpose(0, 3, 2, 1, 4).reshape(EL * JE * 128, D)),
            "wd_s": f(wd[es].reshape(EL, JE, 128, NB, 512).transpose(0, 3, 2, 1, 4).reshape(EL * NB * 128, JE * 512)),
            "bgT": bgT, "buT": buT, "b_dn": b_dn,
        }
        maps.append(m)
    return maps


def run(cfg, inp, stop_after=99, debug=(), trace=False):
    nc = build(cfg, stop_after=stop_after, debug=debug)
    maps = prep_inputs(cfg, inp)
    res = run_bass_kernel_spmd(nc, maps, core_ids=list(range(NCORES)), trace=trace)
    return res


def kernel(**inputs):
    cfg = Cfg()
    res = run(cfg, inputs)
    out = np.zeros((cfg.B, cfg.SEQ, cfg.D), np.float32)
    for core in range(NCORES):
        b, hf = core // 2, core % 2
        out[b, hf * cfg.HALF:(hf + 1) * cfg.HALF] = np.asarray(res.results[core]["out"])
    return out
```
